# Optimizing a Trainium2 kernel written in Bass

```python
import jax, jax.numpy as jnp
from jax import lax
import numpy as np

D_MODEL = 1024
BATCH = 16
SEQ = 2048
DEPTH = 2

CHUNK = 64
D_MIX = D_MODEL
RWKV_HEAD = 64
D_RWKV = D_MIX // 2
RWKV_HEADS = D_RWKV // RWKV_HEAD
W_LORA = 64
A_LORA = 64
V_LORA = 32
G_LORA = 160
D_RWKV_IN = 3 * D_RWKV + W_LORA + A_LORA + G_LORA
RWKV_GN_EPS = 64e-5
D_GLA = D_MIX - D_RWKV
GLA_HEADS = 4
GLA_DV = D_GLA // GLA_HEADS
GLA_DK = GLA_DV // 2
GLA_GATE_RANK = 16
GLA_GATE_NORMALIZER = 16.0
D_GLA_IN = 2 * GLA_HEADS * GLA_DK + D_GLA + GLA_GATE_RANK + D_GLA
D_IN = D_RWKV_IN + D_GLA_IN
RMS_EPS = 1e-6
N_EXPERTS = 32
TOP_K = 4
D_FF = D_MODEL
SWIGLU_LIMIT = 7.0
SWIGLU_ALPHA = 1.702
MOE_BLOCK = 256
DEEPNORM_ALPHA = (2 * DEPTH) ** 0.25
DEEPNORM_BETA = (8 * DEPTH) ** -0.25
LN_EPS = 1e-5

kernel_name = 'hybrid_rwkv7_gla_moe_deepnorm'


def layer_norm(x, w, b):
    xf = x.astype(jnp.float32)
    mu = xf.mean(-1, keepdims=True)
    var = jnp.square(xf - mu).mean(-1, keepdims=True)
    y = (xf - mu) * lax.rsqrt(var + LN_EPS) * w.astype(jnp.float32) + b.astype(jnp.float32)
    return y.astype(x.dtype)


def rwkv7_recurrence(r, decay, k, v, kk, a):
    B, S, H, N = r.shape

    def step(state, inp):
        r_t, w_t, k_t, v_t, kk_t, a_t = inp
        sa = jnp.einsum('bhvk,bhk->bhv', state, -kk_t)
        state = (state * w_t[:, :, None, :]
                 + sa[..., None] * (kk_t * a_t)[:, :, None, :]
                 + v_t[..., None] * k_t[:, :, None, :])
        return state, jnp.einsum('bhvk,bhk->bhv', state, r_t)

    xs = tuple(jnp.moveaxis(t, 1, 0) for t in (r, decay, k, v, kk, a))
    _, y = lax.scan(step, jnp.zeros((B, H, N, N), jnp.float32), xs)
    return jnp.moveaxis(y, 0, 1)


def rwkv7_mixer(p, mu, w0, w_up, a0, a_up, g_up, k_k, k_a, r_k, gn_w, gn_b, v_first, vres):
    B, S, _ = p.shape
    dtype = p.dtype
    prev = jnp.pad(p, ((0, 0), (1, 0), (0, 0)))[:, :S]
    p = p + (prev - p) * mu
    cut = [D_RWKV, 2 * D_RWKV, 3 * D_RWKV, 3 * D_RWKV + W_LORA, 3 * D_RWKV + W_LORA + A_LORA]
    r, k, v, wd, ad, gd = jnp.split(p, cut, axis=-1)
    w = -jax.nn.softplus(-(w0 + jnp.tanh(wd) @ w_up).astype(jnp.float32)) - 0.5
    decay = jnp.exp(-jnp.exp(w))
    a = jax.nn.sigmoid(a0 + ad @ a_up)
    g = jax.nn.sigmoid(gd) @ g_up
    if vres is None:
        v_first = v
    else:
        v0, v_down, v_up = vres
        v = v + (v_first - v) * jax.nn.sigmoid(v0 + (v @ v_down) @ v_up)

    def heads(t):
        return t.reshape(B, S, RWKV_HEADS, RWKV_HEAD).astype(jnp.float32)

    def hp(t):
        return t.reshape(RWKV_HEADS, RWKV_HEAD).astype(jnp.float32)

    r, k, v, decay, a = heads(r), heads(k), heads(v), heads(decay), heads(a)
    kk = k * hp(k_k)
    kk = kk / jnp.maximum(jnp.sqrt(jnp.sum(kk * kk, -1, keepdims=True)), 1e-12)
    k = k * (1.0 + (a - 1.0) * hp(k_a))
    y = rwkv7_recurrence(r, decay, k, v, kk, a)
    m = y.mean(-1, keepdims=True)
    var = jnp.square(y - m).mean(-1, keepdims=True)
    y = (y - m) * lax.rsqrt(var + RWKV_GN_EPS) * hp(gn_w) + hp(gn_b)
    y = y + jnp.sum(r * k * hp(r_k), -1, keepdims=True) * v
    return y.reshape(B, S, D_RWKV).astype(dtype) * g, v_first


def gla_chunked(q, k, v, gk):
    B, S, H, DK = q.shape
    DV = v.shape[-1]
    NC = S // CHUNK

    def blocks(t):
        return t.reshape(B, NC, CHUNK, H, t.shape[-1]).transpose(0, 3, 1, 2, 4)

    q, k, v, gk = blocks(q), blocks(k), blocks(v), blocks(gk)
    b = jnp.cumsum(gk, axis=3)
    b_last = b[:, :, :, -1:, :]
    q_e = q * jnp.exp(b) * (DK ** -0.5)
    k_e = k * jnp.exp(-b)
    k_end = k * jnp.exp(b_last - b)
    causal = jnp.tril(jnp.ones((CHUNK, CHUNK), dtype=bool))
    scores = jnp.where(causal, jnp.einsum('bhncd,bhnsd->bhncs', q_e, k_e), 0.0)
    o_intra = jnp.einsum('bhncs,bhnsv->bhncv', scores, v)
    kv_chunk = jnp.einsum('bhncd,bhncv->bhndv', k_end, v)
    decay_chunk = jnp.exp(b_last[:, :, :, 0, :])

    def step(state, inp):
        kv_n, dec_n = inp
        return state * dec_n[..., None] + kv_n, state

    _, s_prev = lax.scan(step, jnp.zeros((B, H, DK, DV), jnp.float32),
                         (jnp.moveaxis(kv_chunk, 2, 0), jnp.moveaxis(decay_chunk, 2, 0)))
    s_prev = jnp.moveaxis(s_prev, 0, 2)
    o = o_intra + jnp.einsum('bhncd,bhndv->bhncv', q_e, s_prev)
    return o.transpose(0, 2, 3, 1, 4).reshape(B, S, H, DV)


def gla_mixer(p, gk_up, gk_b, norm_w):
    B, S, _ = p.shape
    hk = GLA_HEADS * GLA_DK
    q, k, v, gkd, g = jnp.split(p, [hk, 2 * hk, 2 * hk + D_GLA, 2 * hk + D_GLA + GLA_GATE_RANK], axis=-1)
    gk = jax.nn.log_sigmoid((gkd @ gk_up + gk_b).astype(jnp.float32)) / GLA_GATE_NORMALIZER

    def heads(t, d):
        return t.reshape(B, S, GLA_HEADS, d).astype(jnp.float32)

    o = gla_chunked(heads(q, GLA_DK), heads(k, GLA_DK), heads(v, GLA_DV), heads(gk, GLA_DK))
    o = o * lax.rsqrt(jnp.mean(o * o, -1, keepdims=True) + RMS_EPS) * norm_w.astype(jnp.float32)
    return o.reshape(B, S, D_GLA).astype(p.dtype) * jax.nn.silu(g)


def moe_ffn(x, router_w, router_b, w1, b1, w2, b2):
    B, S, D = x.shape
    T = B * S
    xt = x.reshape(T, D)
    logits = (xt @ router_w + router_b).astype(jnp.float32)
    top_val, top_idx = lax.top_k(logits, TOP_K)
    gate = jax.nn.softmax(top_val, axis=-1)
    M = T * TOP_K
    flat_e = top_idx.reshape(M)
    flat_tok = jnp.arange(M, dtype=jnp.int32) // TOP_K
    flat_gate = gate.reshape(M)
    order = jnp.argsort(flat_e)
    e_sorted = flat_e[order]
    counts = jnp.bincount(flat_e, length=N_EXPERTS)
    padded = ((counts + MOE_BLOCK - 1) // MOE_BLOCK) * MOE_BLOCK
    start = jnp.cumsum(counts) - counts
    pend = jnp.cumsum(padded)
    pstart = pend - padded
    dest = pstart[e_sorted] + jnp.arange(M, dtype=jnp.int32) - start[e_sorted]
    n_blocks = -(-M // MOE_BLOCK) + N_EXPERTS
    m_pad = n_blocks * MOE_BLOCK
    row_tok = jnp.zeros((m_pad,), jnp.int32).at[dest].set(flat_tok[order])
    row_gate = jnp.zeros((m_pad,), jnp.float32).at[dest].set(flat_gate[order])
    block_e = jnp.minimum(
        jnp.searchsorted(pend, jnp.arange(n_blocks, dtype=jnp.int32) * MOE_BLOCK, side='right'),
        N_EXPERTS - 1)

    def body(y, inp):
        tok, g, e = inp
        h = xt[tok] @ w1[e] + b1[e]
        gt = jnp.minimum(h[:, :D_FF], SWIGLU_LIMIT)
        up = jnp.clip(h[:, D_FF:], -SWIGLU_LIMIT, SWIGLU_LIMIT)
        act = (up + 1.0) * gt * jax.nn.sigmoid(SWIGLU_ALPHA * gt)
        o = act @ w2[e] + b2[e]
        return y.at[tok].add(o * g[:, None].astype(o.dtype)), None

    y, _ = lax.scan(body, jnp.zeros_like(xt),
                    (row_tok.reshape(n_blocks, MOE_BLOCK), row_gate.reshape(n_blocks, MOE_BLOCK), block_e))
    return y.reshape(B, S, D)


def setup_inputs(seed: int = 0) -> dict:
    key = jax.random.key(seed)
    ks = iter(jax.random.split(key, 48))

    def nrm(shape, scale):
        return scale * jax.random.normal(next(ks), shape, jnp.float32)

    def uni(shape, lo, hi):
        return jax.random.uniform(next(ks), shape, jnp.float32, lo, hi)

    L = DEPTH
    LV = DEPTH - 1
    return {
        'x': nrm((BATCH, SEQ, D_MODEL), 1.0),
        'ln_in_w': 1.0 + nrm((D_MODEL,), 0.02),
        'ln_in_b': nrm((D_MODEL,), 0.02),
        'w_in': nrm((L, D_MODEL, D_IN), D_MODEL ** -0.5),
        'rwkv_mu': uni((L, D_RWKV_IN), 0.0, 1.0),
        'rwkv_w0': uni((L, D_RWKV), -6.0, 1.0),
        'rwkv_w_up': nrm((L, W_LORA, D_RWKV), W_LORA ** -0.5),
        'rwkv_a0': nrm((L, D_RWKV), 0.1),
        'rwkv_a_up': nrm((L, A_LORA, D_RWKV), A_LORA ** -0.5),
        'rwkv_g_up': nrm((L, G_LORA, D_RWKV), G_LORA ** -0.5),
        'rwkv_k_k': 0.85 + nrm((L, D_RWKV), 0.05),
        'rwkv_k_a': 1.0 + nrm((L, D_RWKV), 0.05),
        'rwkv_r_k': nrm((L, D_RWKV), 0.1),
        'rwkv_gn_w': 1.0 + nrm((L, D_RWKV), 0.02),
        'rwkv_gn_b': nrm((L, D_RWKV), 0.02),
        'rwkv_v0': nrm((LV, D_RWKV), 0.5),
        'rwkv_v_down': nrm((LV, D_RWKV, V_LORA), D_RWKV ** -0.5),
        'rwkv_v_up': nrm((LV, V_LORA, D_RWKV), V_LORA ** -0.5),
        'gla_gk_up': nrm((L, GLA_GATE_RANK, GLA_HEADS * GLA_DK), GLA_GATE_RANK ** -0.5),
        'gla_gk_b': nrm((L, GLA_HEADS * GLA_DK), 0.5),
        'gla_norm_w': 1.0 + nrm((L, GLA_DV), 0.02),
        'w_out': nrm((L, D_MIX, D_MODEL), DEEPNORM_BETA * D_MIX ** -0.5),
        'ln1_w': 1.0 + nrm((L, D_MODEL), 0.02),
        'ln1_b': nrm((L, D_MODEL), 0.02),
        'router_w': nrm((L, D_MODEL, N_EXPERTS), D_MODEL ** -0.5),
        'router_b': nrm((L, N_EXPERTS), 0.01),
        'exp_w1': nrm((L, N_EXPERTS, D_MODEL, 2 * D_FF), D_MODEL ** -0.5),
        'exp_b1': nrm((L, N_EXPERTS, 2 * D_FF), 0.02),
        'exp_w2': nrm((L, N_EXPERTS, D_FF, D_MODEL), DEEPNORM_BETA * D_FF ** -0.5),
        'exp_b2': nrm((L, N_EXPERTS, D_MODEL), 0.02),
        'ln2_w': 1.0 + nrm((L, D_MODEL), 0.02),
        'ln2_b': nrm((L, D_MODEL), 0.02),
    }


def reference(x, ln_in_w, ln_in_b, w_in, rwkv_mu, rwkv_w0, rwkv_w_up, rwkv_a0, rwkv_a_up, rwkv_g_up,
              rwkv_k_k, rwkv_k_a, rwkv_r_k, rwkv_gn_w, rwkv_gn_b, rwkv_v0, rwkv_v_down, rwkv_v_up,
              gla_gk_up, gla_gk_b, gla_norm_w, w_out, ln1_w, ln1_b, router_w, router_b,
              exp_w1, exp_b1, exp_w2, exp_b2, ln2_w, ln2_b):
    x = layer_norm(x, ln_in_w, ln_in_b)
    v_first = None
    for l in range(DEPTH):
        p = x @ w_in[l]
        p_rwkv = p[..., :D_RWKV_IN]
        p_gla = p[..., D_RWKV_IN:]
        vres = None if l == 0 else (rwkv_v0[l - 1], rwkv_v_down[l - 1], rwkv_v_up[l - 1])
        y_rwkv, v_first = rwkv7_mixer(p_rwkv, rwkv_mu[l], rwkv_w0[l], rwkv_w_up[l], rwkv_a0[l],
                                      rwkv_a_up[l], rwkv_g_up[l], rwkv_k_k[l], rwkv_k_a[l],
                                      rwkv_r_k[l], rwkv_gn_w[l], rwkv_gn_b[l], v_first, vres)
        y_gla = gla_mixer(p_gla, gla_gk_up[l], gla_gk_b[l], gla_norm_w[l])
        mix = jnp.concatenate([y_rwkv, y_gla], axis=-1) @ w_out[l]
        x = layer_norm(DEEPNORM_ALPHA * x + mix, ln1_w[l], ln1_b[l])
        ffn = moe_ffn(x, router_w[l], router_b[l], exp_w1[l], exp_b1[l], exp_w2[l], exp_b2[l])
        x = layer_norm(DEEPNORM_ALPHA * x + ffn, ln2_w[l], ln2_b[l])
    return x
```

```python
from contextlib import ExitStack
import numpy as np
import concourse.bass as bass
import concourse.mybir as mybir
from concourse.bass_utils import run_bass_kernel_spmd

F32 = mybir.dt.float32
BF16 = mybir.dt.bfloat16
AF = mybir.ActivationFunctionType
ALU = mybir.AluOpType
AX = mybir.AxisListType

ENGS = ['pe', 'act', 'dve', 'pool', 'sp']
NDMA_SEM = 12
import os as _os
SKIPW = bool(_os.environ.get('K_SKIPW'))
NCORES = 8
TOK = 4096
SEQ = 2048
D = 1024
DIN = 3376
NE = 32
ALPHA = 4.0 ** 0.25
LN_EPS = 1e-5
GN_EPS = 64e-5
RMS_EPS = 1e-6
C_W = -float(np.exp(-0.5))
C7 = float(1.0 / (1.0 + np.exp(-1.702 * 7.0)))


class Op:
    __slots__ = ('eng', 'fn', 'deps', 'signal', 'sigval', 'is_dma', 'didx')

    def __init__(self, eng, fn, is_dma):
        self.eng = eng
        self.fn = fn
        self.is_dma = is_dma
        self.deps = []
        self.signal = False
        self.sigval = 0
        self.didx = -1


class Sched:
    def __init__(self):
        self.ops = {e: [] for e in ENGS}
        self.lastw = {}
        self.readers = {}
        self.dmas_since_barrier = []

    def add(self, eng, fn, reads=(), writes=(), dma=False, extra=()):
        op = Op(eng, fn, dma)
        deps = {}
        for k in reads:
            w = self.lastw.get(k)
            if w is not None:
                deps[id(w)] = w
        for k in writes:
            w = self.lastw.get(k)
            if w is not None:
                deps[id(w)] = w
            for r in self.readers.get(k, ()):
                deps[id(r)] = r
        for d in extra:
            deps[id(d)] = d
        op.deps = list(deps.values())
        for k in reads:
            self.readers.setdefault(k, []).append(op)
        for k in writes:
            self.lastw[k] = op
            self.readers[k] = []
        self.ops[eng].append(op)
        if dma:
            self.dmas_since_barrier.append(op)
        return op

    def barrier(self):
        last = []
        for e in ENGS:
            for op in reversed(self.ops[e]):
                if op.fn is not None and not op.is_dma:
                    last.append(op)
                    break
        deps = last + self.dmas_since_barrier
        for e in ENGS:
            self.add(e, None, extra=deps)
        self.lastw = {}
        self.readers = {}
        self.dmas_since_barrier = []

    def _needs_wait(self, op, d):
        if d.is_dma:
            return True
        if d.eng != op.eng:
            return True
        if op.eng == 'pe':
            return False
        return True

    def emit(self, block, esem, dsems):
        for e in ENGS:
            for op in self.ops[e]:
                for d in op.deps:
                    if not d.is_dma and self._needs_wait(op, d):
                        d.signal = True
        for e in ENGS:
            c = 0
            k = 0
            for op in self.ops[e]:
                if op.is_dma:
                    op.didx = k
                    k += 1
                elif op.signal:
                    c += 1
                    op.sigval = c
        sched = self

        def target(d):
            if d.is_dma:
                return dsems[d.eng][d.didx % NDMA_SEM], 16 * (d.didx // NDMA_SEM + 1)
            return esem[d.eng], d.sigval

        def run(e, h):
            known = {}
            for op in sched.ops[e]:
                for d in op.deps:
                    if not sched._needs_wait(op, d):
                        continue
                    sem, val = target(d)
                    if known.get(id(sem), 0) < val:
                        h.wait_ge(sem, val)
                        known[id(sem)] = val
                if op.is_dma:
                    sem = dsems[e][op.didx % NDMA_SEM]
                    prev = 16 * (op.didx // NDMA_SEM)
                    if prev > 0 and known.get(id(sem), 0) < prev:
                        h.wait_ge(sem, prev)
                        known[id(sem)] = prev
                    op.fn(h).then_inc(sem, 16)
                elif op.fn is not None:
                    ins = op.fn(h)
                    if op.signal:
                        ins.then_inc(esem[e], 1)

        @block.tensor
        def _(h):
            run('pe', h)

        @block.scalar
        def _(h):
            run('act', h)

        @block.vector
        def _(h):
            run('dve', h)

        @block.gpsimd
        def _(h):
            run('pool', h)

        @block.sync
        def _(h):
            run('sp', h)


def _colpack64(v, ncol):
    v = np.asarray(v, np.float32).reshape(ncol, 64)
    out = np.zeros((128, ncol), np.float32)
    out[0:64, :] = v.T
    return out


def _colpack(v, ncol):
    v = np.asarray(v, np.float32).reshape(-1)
    out = np.zeros((ncol * 128,), np.float32)
    out[:v.size] = v
    return np.ascontiguousarray(out.reshape(ncol, 128).T)


class ParamLayout:
    def __init__(self):
        self.off = {}
        self.n = 0

    def add(self, name, ncol):
        self.off[name] = self.n
        self.n += ncol


def make_layout():
    L = ParamLayout()
    L.add('ln_in_w', 8)
    L.add('ln_in_b', 8)
    L.add('c_lneps', 1)
    L.add('c_gneps', 1)
    L.add('c_rmseps', 1)
    L.add('c_one', 1)
    for l in range(2):
        for nm in ['ln1_w', 'ln1_b', 'ln2_w', 'ln2_b']:
            L.add(f'{nm}{l}', 8)
        for nm in ['mu_r', 'mu_k', 'mu_v', 'w0', 'a0', 'k_k', 'k_a', 'r_k', 'gn_w', 'gn_b', 'v0']:
            L.add(f'{nm}{l}', 8)
        L.add(f'mu_wd{l}', 1)
        L.add(f'mu_ad{l}', 1)
        L.add(f'mu_gd{l}', 2)
        L.add(f'ngk_b{l}', 4)
        L.add(f'gnorm_w{l}', 1)
        L.add(f'b1{l}', 512)
    return L


def pack_params(inp, L):
    P = np.zeros((128, L.n), np.float32)

    def put(name, arr):
        o = L.off[name]
        P[:, o:o + arr.shape[1]] = arr

    put('ln_in_w', _colpack(inp['ln_in_w'], 8))
    put('ln_in_b', _colpack(inp['ln_in_b'], 8))
    P[:, L.off['c_lneps']] = LN_EPS
    P[:, L.off['c_gneps']] = GN_EPS
    P[:, L.off['c_rmseps']] = RMS_EPS
    P[:, L.off['c_one']] = 1.0
    for l in range(2):
        for nm in ['ln1_w', 'ln1_b', 'ln2_w', 'ln2_b']:
            put(f'{nm}{l}', _colpack(inp[nm][l], 8))
        mu = inp['rwkv_mu'][l]
        put(f'mu_r{l}', _colpack64(mu[0:512], 8))
        put(f'mu_k{l}', _colpack64(mu[512:1024], 8))
        put(f'mu_v{l}', _colpack64(mu[1024:1536], 8))
        put(f'mu_wd{l}', _colpack(mu[1536:1600], 1))
        put(f'mu_ad{l}', _colpack(mu[1600:1664], 1))
        put(f'mu_gd{l}', _colpack(mu[1664:1824], 2))
        for nm, src in [('w0', 'rwkv_w0'), ('a0', 'rwkv_a0'), ('k_k', 'rwkv_k_k'), ('k_a', 'rwkv_k_a'),
                        ('r_k', 'rwkv_r_k'), ('gn_w', 'rwkv_gn_w'), ('gn_b', 'rwkv_gn_b')]:
            put(f'{nm}{l}', _colpack64(inp[src][l], 8))
        if l >= 1:
            put(f'v0{l}', _colpack64(inp['rwkv_v0'][l - 1], 8))
        gb = np.zeros((128, 4), np.float32)
        gb[0:64, :] = np.asarray(inp['gla_gk_b'][l], np.float32).reshape(4, 64).T
        put(f'ngk_b{l}', gb)
        put(f'gnorm_w{l}', _colpack(inp['gla_norm_w'][l], 1))
        b1 = np.asarray(inp['exp_b1'][l], np.float32).reshape(NE, 16, 128)
        put(f'b1{l}', np.ascontiguousarray(b1.transpose(2, 0, 1).reshape(128, 512)))
    return P


def make_consts():
    ident = np.eye(128, dtype=np.float32)
    blockones = np.zeros((128, 128), np.float32)
    blockones[0:64, 0:64] = 1.0
    blockones[64:128, 64:128] = 1.0
    onesd = np.full((128, 128), 1.0 / D, np.float32)
    i = np.arange(128)[:, None]
    t = np.arange(128)[None, :]
    same = (i // 64) == (t // 64)
    strict = ((i < t) & same).astype(np.float32)
    incl = ((i <= t) & same).astype(np.float32)
    lower = ((i > t) & same).astype(np.float32)
    mask4 = np.concatenate([strict, strict, incl, incl], 1)
    keep = np.ones((128, 512), np.float32)
    keep[:, ::64] = 0.0
    m4c = np.zeros((128, 256), np.float32)
    m4c[0:64] = np.concatenate([strict[0:64, 0:64], strict[0:64, 0:64], incl[0:64, 0:64], incl[0:64, 0:64]], 1)
    return np.ascontiguousarray(np.concatenate([ident, blockones, onesd, mask4, lower, incl, keep, m4c], 1))


CO = {'ident': 0, 'bones': 128, 'onesd': 256, 'mask4': 384, 'lower': 896, 'incl': 1024, 'keep': 1152, 'm4c': 1664}
NCONST = 1920


def build(debug=False, stop=None):
    L = make_layout()
    nc = bass.Bass("TRN2", target_bir_lowering=False)
    dk = "ExternalOutput" if debug else "Internal"

    def dram(name, shape, dt=F32, kind="ExternalInput"):
        return nc.dram_tensor(name, shape, dt, kind=kind).ap()

    xT = dram("xT", [D, TOK])
    params_d = dram("params", [128, L.n])
    consts_d = dram("consts", [128, NCONST])
    w_in = dram("w_in", [2, D, DIN])
    w_upd = dram("rwkv_w_up", [2, 64, 512])
    a_upd = dram("rwkv_a_up", [2, 64, 512])
    g_upd = dram("rwkv_g_up", [2, 160, 512])
    v_downd = dram("rwkv_v_down", [1, 512, 32])
    v_upd = dram("rwkv_v_up", [1, 32, 512])
    gk_upd = dram("gla_gk_up", [2, 16, 256])
    w_out = dram("w_out", [2, D, D])
    router_w = dram("router_w", [2, D, NE])
    router_bb = dram("router_bb", [2, 128, NE])
    exp_w1 = dram("exp_w1", [2, NE, D, 2 * D])
    exp_w2 = dram("exp_w2", [2, NE, D, D])
    exp_b2 = dram("exp_b2", [2, NE, D])
    outT = dram("outT", [D, TOK], kind="ExternalOutput")
    xres = dram("xres", [D, TOK], F32, dk)
    x16 = dram("x16", [D, TOK], BF16, dk)
    ymix = dram("ymix", [D, TOK], BF16, dk)
    vfirst = dram("vfirst", [512, TOK], F32, "Internal")
    gTd = dram("gTd", [NE, TOK], F32, dk)

    S = Sched()
    es = ExitStack()
    with es:
        AW = 53000
        arena = es.enter_context(nc.sbuf_tensor("arena", [128, AW], F32))
        banks = [es.enter_context(nc.psum_tensor(f"ps{i}", [128, 512], F32)) for i in range(8)]
        esem = {e: es.enter_context(nc.semaphore("s_" + e)) for e in ENGS}
        dsems = {e: [es.enter_context(nc.semaphore(f"d_{e}{i}")) for i in range(NDMA_SEM)] for e in ['sp', 'pool']}
        block = es.enter_context(nc.Block())

        st = {'top': 0, 'mark': 0, 'uid': 0}

        def alloc(words, name=None):
            o = st['top']
            st['top'] += words
            assert st['top'] <= AW, f"arena overflow {st['top']}"
            st['uid'] += 1
            return o, (name or 'a') + str(st['uid'])

        class Buf:
            def __init__(self, words, name=None, dt=F32):
                self.o, self.key = alloc(words, name)
                self.words = words
                self.dt = dt

            def ap(self, p0=0, p1=128):
                a = arena[p0:p1, self.o:self.o + self.words]
                return a.bitcast(BF16) if self.dt == BF16 else a

        def pcol(name, c=0, p0=0, p1=128):
            o = PARB.o + L.off[name] + c
            return arena[p0:p1, o:o + 1]

        def cst(name, w, p0=0, p1=128):
            o = CONB.o + CO[name]
            return arena[p0:p1, o:o + w]

        psi = {'i': 0, 'y': 0, 'n': 6}

        def pb():
            i = psi['i'] % psi['n']
            psi['i'] += 1
            return banks[i], f'ps{i}'

        def ybank():
            i = 6 + psi['y'] % 2
            psi['y'] += 1
            return banks[i], f'ps{i}'

        def op(eng, name, *args, r=(), w=(), **kw):
            return S.add(eng, lambda h: getattr(h, name)(*args, **kw), reads=r, writes=w)

        def dma(q, out, in_, r=(), w=()):
            return S.add(q, lambda h: h.dma_start(out=out, in_=in_), reads=r, writes=w, dma=True)

        def mm(out, lhsT, rhs, start, stop, r, w):
            return S.add('pe', lambda h: h.matmul(out, lhsT=lhsT, rhs=rhs, start=start, stop=stop), reads=r, writes=w)

        PARB = Buf(L.n, 'params')
        CONB = Buf(NCONST, 'consts')
        dma('sp', PARB.ap(), params_d, w=[PARB.key])
        dma('sp', CONB.ap(), consts_d, w=[CONB.key])
        CK = [PARB.key, CONB.key]
        ident = cst('ident', 128)
        RST = Buf(8 * 64, 'rstate')
        GST = Buf(4 * 128, 'gstate')
        PRV = Buf(32, 'prevcol')
        st['mark'] = st['top']

        def phase_reset():
            S.barrier()
            st['top'] = st['mark']

        def layer_norm_tile(z3, zflat, zk, wname, bname, tok0, scr, to_out=False):
            SQ, T1, T2 = scr
            sq3 = SQ.ap().rearrange("p (c t) -> p c t", c=8)
            if zflat is not None:
                op('act', 'activation', out=SQ.ap(), in_=zflat, func=AF.Square, r=[zk], w=[SQ.key])
            else:
                op('act', 'activation', out=sq3, in_=z3, func=AF.Square, r=[zk], w=[SQ.key])
            bm, km = pb()
            for c in range(8):
                mm(bm[:], cst('onesd', 128), z3[:, c, :], c == 0, c == 7, [zk] + CK, [km])
            bq, kq = pb()
            for c in range(8):
                mm(bq[:], cst('onesd', 128), sq3[:, c, :], c == 0, c == 7, [SQ.key] + CK, [kq])
            op('act', 'activation', out=T1.ap(), in_=bm[:], func=AF.Copy, r=[km], w=[T1.key])
            op('dve', 'tensor_tensor', out=T2.ap(), in0=T1.ap(), in1=T1.ap(), op=ALU.mult, r=[T1.key], w=[T2.key])
            op('dve', 'tensor_tensor', out=T2.ap(), in0=bq[:], in1=T2.ap(), op=ALU.subtract, r=[kq, T2.key], w=[T2.key])
            op('act', 'activation', out=T2.ap(), in_=T2.ap(), func=AF.Sqrt, bias=pcol('c_lneps'), scale=1.0,
               r=[T2.key] + CK, w=[T2.key])
            op('dve', 'reciprocal', out=T2.ap(), in_=T2.ap(), r=[T2.key], w=[T2.key])
            x16v = arena[:, SQ.o:SQ.o + 2048].bitcast(BF16).rearrange("p (c t) -> p c t", c=8)
            zc = [(zk, 'c', c) for c in range(8)]
            for c in range(8):
                op('dve', 'tensor_tensor', out=z3[:, c, :], in0=z3[:, c, :], in1=T1.ap(), op=ALU.subtract,
                   r=[zk, T1.key], w=[zc[c]])
            for c in range(8):
                op('dve', 'tensor_tensor', out=z3[:, c, :], in0=z3[:, c, :], in1=T2.ap(), op=ALU.mult,
                   r=[zc[c], T2.key], w=[zc[c]])
                op('act', 'activation', out=z3[:, c, :], in_=z3[:, c, :], func=AF.Identity,
                   bias=pcol(bname, c), scale=pcol(wname, c), r=[zc[c]] + CK, w=[zc[c]])
            if to_out:
                dma('sp', outT.rearrange("(c p) t -> p c t", p=128)[:, :, tok0:tok0 + 512], z3, r=[zk] + zc, w=[('out', tok0)])
            else:
                op('pool', 'tensor_copy', out=x16v, in_=z3, r=[zk] + zc, w=[SQ.key])
                dma('sp', xres.rearrange("(c p) t -> p c t", p=128)[:, :, tok0:tok0 + 512], z3, r=[zk] + zc, w=[('xres', tok0)])
                dma('sp', x16.rearrange("(c p) t -> p c t", p=128)[:, :, tok0:tok0 + 512], x16v, r=[SQ.key], w=[('x16', tok0)])

        def finish():
            S.add('sp', None, reads=[('out', t * 512) for t in range(8)])
            S.emit(block, esem, dsems)
            return nc

        Zb = [Buf(4096, 'Z'), Buf(4096, 'Z')]
        SQb = [Buf(4096, 'SQ'), Buf(4096, 'SQ')]
        T1b = [Buf(512, 'T1'), Buf(512, 'T1')]
        T2b = [Buf(512, 'T2'), Buf(512, 'T2')]
        for tt in range(8):
            Z = Zb[tt % 2]
            tok0 = tt * 512
            dma('sp', Z.ap().rearrange("p (c t) -> p c t", c=8),
                xT.rearrange("(c p) t -> p c t", p=128)[:, :, tok0:tok0 + 512], w=[Z.key])
            layer_norm_tile(Z.ap().rearrange("p (c t) -> p c t", c=8), Z.ap(), Z.key, 'ln_in_w', 'ln_in_b', tok0, (SQb[tt % 2], T1b[tt % 2], T2b[tt % 2]))
        if stop == 'ln_in':
            return finish()

        for l in range(2):
            phase_reset()
            psi['n'] = 6
            WIN = Buf(8 * DIN // 2, 'win', BF16)
            win3 = WIN.ap().rearrange("p (c f) -> p c f", c=8)
            dma('pool', win3, w_in[l].rearrange("(c p) f -> p c f", p=128), w=[WIN.key])
            WUP = Buf(512, 'wup')
            AUP = Buf(512, 'aup')
            GUP = Buf(1024, 'gup')
            VDN = Buf(256, 'vdn')
            VUP = Buf(512, 'vup')
            GKU = Buf(256, 'gku')
            dma('sp', WUP.ap(0, 64), w_upd[l], w=[WUP.key])
            dma('sp', AUP.ap(0, 64), a_upd[l], w=[AUP.key])
            dma('sp', GUP.ap()[:, 0:512], g_upd[l, 0:128, :], w=[GUP.key])
            dma('sp', GUP.ap(0, 32)[:, 512:1024], g_upd[l, 128:160, :], w=[GUP.key])
            dma('sp', GKU.ap(0, 16), gk_upd[l], w=[GKU.key])
            if l >= 1:
                dma('sp', VDN.ap(0, 64).rearrange("p (c f) -> p c f", c=8),
                    v_downd[l - 1].rearrange("(c p) f -> p c f", p=64), w=[VDN.key])
                dma('sp', VUP.ap(0, 32), v_upd[l - 1], w=[VUP.key])

            XB = [Buf(2048, 'xb', BF16), Buf(2048, 'xb', BF16)]
            PST = Buf(513, 'pst')
            names = ['vall0', 'vall1', 'vall2', 'vall3', 'vall4', 'vall5', 'vall6', 'vall7', 'twd', 'adT', 'sgd0', 'sgd1', 'gkd',
                     'r', 'k', 'g', 'lw', 'cum', 'a', 'kk', 'be', 'e1', 'e2', 'rt', 'kt', 'bt', 'at', 'bend', 'kend', 'bv', 'ynT',
                     'vf', 'vd', 'ys', 'sq']
            W = {n: Buf(512, n) for n in names}
            PCB = Buf(8, 'pc')
            TM = [Buf(192, 'tm') for _ in range(8)]
            AM = [Buf(256, 'am') for _ in range(8)]
            NQ = [[Buf(128, 'nq'), Buf(128, 'nq')] for _ in range(8)]
            TT = [[Buf(64, 'tt'), Buf(64, 'tt')] for _ in range(8)]
            W0B = [Buf(64, 'w0b') for _ in range(2)]
            UB = [Buf(64, 'ub') for _ in range(2)]
            STA = Buf(32, 'sta')
            OUT16 = [Buf(256, 'o16', BF16) for _ in range(2)]
            H = 64
            id64 = arena[0:64, CONB.o + CO['ident']:CONB.o + CO['ident'] + 64]

            def tr(out_, in_, idn, r, w):
                return S.add('pe', lambda h: h.transpose(out=out_, in_=in_, identity=idn), reads=r, writes=w)

            def proj(xb, col0, M, dst_ap, dst_key):
                bk, kk_ = pb()
                for c in range(8):
                    mm(bk[0:M, :], win3[:, c, col0:col0 + M], xb.ap().rearrange("p (c t) -> p c t", c=8)[:, c, :],
                       c == 0, c == 7, [xb.key, WIN.key], [kk_])
                op('act', 'activation', out=dst_ap, in_=bk[0:M, :], func=AF.Copy, r=[kk_], w=[dst_key])

            def proj_shift(xb, col0, M, dst, muname, mucol, slot, first):
                pst = PST.ap(0, M)
                prv = PRV.ap(0, M)[:, slot:slot + 1]
                if first:
                    op('dve', 'memset', pst[:, 0:1], 0.0, w=[PST.key])
                else:
                    op('dve', 'tensor_copy', out=pst[:, 0:1], in_=prv, r=[PRV.key], w=[PST.key])
                proj(xb, col0, M, pst[:, 1:513], PST.key)
                op('dve', 'tensor_copy', out=prv, in_=pst[:, 512:513], r=[PST.key], w=[PRV.key])
                d = dst.ap(0, M)
                op('dve', 'tensor_tensor', out=d, in0=pst[:, 0:512], in1=pst[:, 1:513], op=ALU.subtract,
                   r=[PST.key], w=[dst.key])
                op('dve', 'scalar_tensor_tensor', out=d, in0=d, scalar=pcol(muname, mucol, 0, M), in1=pst[:, 1:513],
                   op0=ALU.mult, op1=ALU.add, r=[dst.key, PST.key] + CK, w=[dst.key])

            def A_(n, p1=H):
                return W[n].ap(0, p1)

            def K_(*ns):
                return [W[n].key for n in ns]

            for b in range(2):
                for blk in range(4):
                    tok0 = b * SEQ + blk * 512
                    first = blk == 0
                    xb = XB[(b * 4 + blk) % 2]
                    dma('sp', xb.ap().rearrange("p (c t) -> p c t", c=8),
                        x16.rearrange("(c p) t -> p c t", p=128)[:, :, tok0:tok0 + 512],
                        r=[('x16', tok0)], w=[xb.key])
                    if first:
                        op('dve', 'memset', RST.ap(), 0.0, w=[RST.key])
                        op('dve', 'memset', GST.ap(), 0.0, w=[GST.key])
                    proj_shift(xb, 1536, 64, W['twd'], f'mu_wd{l}', 0, 24, first)
                    op('act', 'activation', out=A_('twd'), in_=A_('twd'), func=AF.Tanh, r=K_('twd'), w=K_('twd'))
                    proj_shift(xb, 1600, 64, W['adT'], f'mu_ad{l}', 0, 25, first)
                    proj_shift(xb, 1664, 128, W['sgd0'], f'mu_gd{l}', 0, 26, first)
                    op('act', 'activation', out=W['sgd0'].ap(), in_=W['sgd0'].ap(), func=AF.Sigmoid, r=K_('sgd0'), w=K_('sgd0'))
                    proj_shift(xb, 1792, 32, W['sgd1'], f'mu_gd{l}', 1, 27, first)
                    op('act', 'activation', out=A_('sgd1', 32), in_=A_('sgd1', 32), func=AF.Sigmoid, r=K_('sgd1'), w=K_('sgd1'))
                    proj(xb, 2848, 16, A_('gkd', 16), W['gkd'].key)
                    for h_ in range(8):
                        proj_shift(xb, 1024 + h_ * 64, 64, W[f'vall{h_}'], f'mu_v{l}', h_, 16 + h_, first)
                    if l == 0:
                        for h_ in range(8):
                            dma('sp', vfirst[h_ * 64:(h_ + 1) * 64, tok0:tok0 + 512], A_(f'vall{h_}'),
                                r=K_(f'vall{h_}'), w=[('vf', h_, tok0)])
                    else:
                        bk, kk_ = pb()
                        for h_ in range(8):
                            mm(bk[0:32, :], VDN.ap(0, 64)[:, h_ * 32:(h_ + 1) * 32], A_(f'vall{h_}'), h_ == 0, h_ == 7,
                               [VDN.key] + K_(f'vall{h_}'), [kk_])
                        op('act', 'activation', out=A_('vd', 32), in_=bk[0:32, :], func=AF.Copy, r=[kk_], w=K_('vd'))
                        for h_ in range(8):
                            va = f'vall{h_}'
                            dma('sp', A_('vf'), vfirst[h_ * 64:(h_ + 1) * 64, tok0:tok0 + 512], w=K_('vf'))
                            bk, kk_ = pb()
                            mm(bk[0:64, :], VUP.ap(0, 32)[:, h_ * 64:(h_ + 1) * 64], A_('vd', 32), True, True, [VUP.key] + K_('vd'), [kk_])
                            op('act', 'activation', out=A_('e1'), in_=bk[0:64, :], func=AF.Sigmoid, bias=pcol(f'v0{l}', h_, 0, 64),
                               scale=1.0, r=[kk_] + CK, w=K_('e1'))
                            op('dve', 'tensor_tensor', out=A_('vf'), in0=A_('vf'), in1=A_(va), op=ALU.subtract, r=K_('vf', va), w=K_('vf'))
                            op('dve', 'tensor_tensor', out=A_('vf'), in0=A_('vf'), in1=A_('e1'), op=ALU.mult, r=K_('vf', 'e1'), w=K_('vf'))
                            op('dve', 'tensor_tensor', out=A_(va), in0=A_(va), in1=A_('vf'), op=ALU.add, r=K_('vf', va), w=K_(va))
                    if stop == 'mixA':
                        return finish()

                    for h_ in range(8):
                        vn = f'vall{h_}'
                        hs = slice(h_ * 64, (h_ + 1) * 64)
                        pc = lambda nm: pcol(f'{nm}{l}', h_, 0, 64)
                        proj_shift(xb, h_ * 64, 64, W['r'], f'mu_r{l}', h_, h_, first)
                        proj_shift(xb, 512 + h_ * 64, 64, W['k'], f'mu_k{l}', h_, 8 + h_, first)
                        bk, kk_ = pb()
                        mm(bk[0:64, :], WUP.ap(0, 64)[:, hs], A_('twd'), True, True, [WUP.key] + K_('twd'), [kk_])
                        op('act', 'activation', out=A_('lw'), in_=bk[0:64, :], func=AF.Sigmoid, bias=pc('w0'), scale=1.0,
                           r=[kk_] + CK, w=K_('lw'))
                        op('dve', 'tensor_scalar', out=A_('lw'), in0=A_('lw'), scalar1=C_W, scalar2=None, op0=ALU.mult, r=K_('lw'), w=K_('lw'))
                        op('dve', 'tensor_tensor_scan', out=A_('cum'), data0=cst('keep', 512, 0, 64), data1=A_('lw'),
                           initial=0.0, op0=ALU.mult, op1=ALU.add, r=K_('lw') + CK, w=K_('cum'))
                        bk, kk_ = pb()
                        mm(bk[0:64, :], AUP.ap(0, 64)[:, hs], A_('adT'), True, True, [AUP.key] + K_('adT'), [kk_])
                        op('act', 'activation', out=A_('a'), in_=bk[0:64, :], func=AF.Sigmoid, bias=pc('a0'), scale=1.0, r=[kk_] + CK, w=K_('a'))
                        bk, kk_ = pb()
                        mm(bk[0:64, :], GUP.ap()[:, hs], W['sgd0'].ap(), True, False, [GUP.key] + K_('sgd0'), [kk_])
                        mm(bk[0:64, :], GUP.ap(0, 32)[:, 512 + h_ * 64:512 + (h_ + 1) * 64], A_('sgd1', 32), False, True,
                           [GUP.key] + K_('sgd1'), [kk_])
                        op('act', 'activation', out=A_('g'), in_=bk[0:64, :], func=AF.Copy, r=[kk_], w=K_('g'))
                        op('dve', 'tensor_scalar', out=A_('kk'), in0=A_('k'), scalar1=pc('k_k'), scalar2=None, op0=ALU.mult,
                           r=K_('k') + CK, w=K_('kk'))
                        op('act', 'activation', out=A_('e1'), in_=A_('kk'), func=AF.Square, r=K_('kk'), w=K_('e1'))
                        bk, kk_ = pb()
                        mm(bk[0:64, :], cst('bones', 64, 0, 64), A_('e1'), True, True, K_('e1') + CK, [kk_])
                        op('act', 'activation', out=A_('e2'), in_=bk[0:64, :], func=AF.Sqrt, r=[kk_], w=K_('e2'))
                        op('dve', 'tensor_scalar', out=A_('e2'), in0=A_('e2'), scalar1=1e-12, scalar2=None, op0=ALU.max, r=K_('e2'), w=K_('e2'))
                        op('dve', 'reciprocal', out=A_('e2'), in_=A_('e2'), r=K_('e2'), w=K_('e2'))
                        op('dve', 'tensor_tensor', out=A_('kk'), in0=A_('kk'), in1=A_('e2'), op=ALU.mult, r=K_('kk', 'e2'), w=K_('kk'))
                        op('dve', 'tensor_scalar', out=A_('e1'), in0=A_('a'), scalar1=-1.0, scalar2=pc('k_a'), op0=ALU.add, op1=ALU.mult,
                           r=K_('a') + CK, w=K_('e1'))
                        op('dve', 'scalar_tensor_tensor', out=A_('k'), in0=A_('e1'), scalar=1.0, in1=A_('k'), op0=ALU.add, op1=ALU.mult,
                           r=K_('e1', 'k'), w=K_('k'))
                        op('dve', 'tensor_tensor', out=A_('be'), in0=A_('kk'), in1=A_('a'), op=ALU.mult, r=K_('kk', 'a'), w=K_('be'))
                        op('dve', 'scalar_tensor_tensor', out=A_('e1'), in0=A_('r'), scalar=pc('r_k'), in1=A_('k'), op0=ALU.mult, op1=ALU.mult,
                           r=K_('r', 'k') + CK, w=K_('e1'))
                        bk, kk_ = pb()
                        mm(bk[0:64, :], cst('bones', 64, 0, 64), A_('e1'), True, True, K_('e1') + CK, [kk_])
                        op('dve', 'tensor_tensor', out=A_('bv'), in0=bk[0:64, :], in1=A_(vn), op=ALU.mult, r=[kk_] + K_(vn), w=K_('bv'))
                        cum3 = A_('cum').rearrange("p (c k) -> p c k", k=64)
                        op('act', 'activation', out=A_('e1'), in_=A_('cum'), func=AF.Exp, r=K_('cum'), w=K_('e1'))
                        op('dve', 'tensor_tensor', out=A_('rt'), in0=A_('r'), in1=A_('e1'), op=ALU.mult, r=K_('r', 'e1'), w=K_('rt'))
                        op('act', 'activation', out=PCB.ap(0, 64), in_=cum3[:, :, 63], func=AF.Exp, r=K_('cum'), w=[PCB.key])
                        op('act', 'activation', out=A_('e2'), in_=A_('cum'), func=AF.Exp, scale=-1.0, r=K_('cum'), w=K_('e2'))
                        op('dve', 'tensor_tensor', out=A_('kt'), in0=A_('k'), in1=A_('e2'), op=ALU.mult, r=K_('k', 'e2'), w=K_('kt'))
                        op('dve', 'tensor_tensor', out=A_('bt'), in0=A_('be'), in1=A_('e2'), op=ALU.mult, r=K_('be', 'e2'), w=K_('bt'))
                        op('dve', 'tensor_tensor', out=A_('e1'), in0=A_('cum'), in1=A_('lw'), op=ALU.subtract, r=K_('cum', 'lw'), w=K_('e1'))
                        op('act', 'activation', out=A_('e1'), in_=A_('e1'), func=AF.Exp, r=K_('e1'), w=K_('e1'))
                        op('dve', 'scalar_tensor_tensor', out=A_('at'), in0=A_('kk'), scalar=-1.0, in1=A_('e1'), op0=ALU.mult, op1=ALU.mult,
                           r=K_('kk', 'e1'), w=K_('at'))
                        e23 = A_('e2').rearrange("p (c k) -> p c k", k=64)
                        op('dve', 'tensor_tensor', out=e23, in0=cum3[:, :, 63:64].to_broadcast([64, 8, 64]), in1=cum3, op=ALU.subtract,
                           r=K_('cum'), w=K_('e2'))
                        op('act', 'activation', out=A_('e2'), in_=A_('e2'), func=AF.Exp, r=K_('e2'), w=K_('e2'))
                        op('dve', 'tensor_tensor', out=A_('bend'), in0=A_('be'), in1=A_('e2'), op=ALU.mult, r=K_('be', 'e2'), w=K_('bend'))
                        op('dve', 'tensor_tensor', out=A_('kend'), in0=A_('k'), in1=A_('e2'), op=ALU.mult, r=K_('k', 'e2'), w=K_('kend'))
                        PREP = K_('rt', 'kt', 'bt', 'at', 'bend', 'kend', vn)
                        if stop == 'mixB':
                            return finish()

                        for c in range(8):
                            cs = slice(c * 64, (c + 1) * 64)
                            bk, kk_ = pb()
                            bt_, kt_, at_, rt_ = A_('bt')[:, cs], A_('kt')[:, cs], A_('at')[:, cs], A_('rt')[:, cs]
                            mm(bk[0:64, 0:64], bt_, at_, True, True, PREP, [kk_])
                            mm(bk[0:64, 64:128], kt_, at_, True, True, PREP, [kk_])
                            mm(bk[0:64, 128:192], bt_, rt_, True, True, PREP, [kk_])
                            mm(bk[0:64, 192:256], kt_, rt_, True, True, PREP, [kk_])
                            mm(bk[0:64, 256:320], at_, bt_, True, True, PREP, [kk_])
                            for i, src in enumerate([vn, 'bend', 'kend']):
                                tr(bk[0:64, 320 + i * 64:384 + i * 64], A_(src)[:, cs], id64, K_(src) + CK, [kk_])
                            op('dve', 'tensor_tensor', out=AM[c].ap(0, 64), in0=bk[0:64, 0:256], in1=cst('m4c', 256, 0, 64), op=ALU.mult,
                               r=[kk_] + CK, w=[AM[c].key])
                            if stop == 'mixB1':
                                return finish()
                            op('dve', 'tensor_tensor', out=NQ[c][0].ap(0, 64)[:, 0:64], in0=bk[0:64, 256:320], in1=cst('lower', 64, 0, 64),
                               op=ALU.mult, r=[kk_] + CK, w=[NQ[c][0].key])
                            if stop == 'mixB2a':
                                return finish()
                            op('dve', 'tensor_copy', out=TM[c].ap(0, 64), in_=bk[0:64, 320:512], r=[kk_], w=[TM[c].key])
                            if stop == 'mixB2':
                                return finish()
                            op('dve', 'tensor_tensor', out=TT[c][0].ap(0, 64), in0=AM[c].ap(0, 64)[:, 0:64], in1=id64, op=ALU.add,
                               r=[AM[c].key] + CK, w=[TT[c][0].key])
                        if stop == 'mixC0':
                            return finish()
                        for lev in range(1, 6):
                            for c in range(8):
                                qo, qn = NQ[c][(lev - 1) % 2], NQ[c][lev % 2]
                                to, tn = TT[c][(lev - 1) % 2], TT[c][lev % 2]
                                Q, Qt = qo.ap(0, 64)[:, 0:64], qo.ap(0, 64)[:, 64:128]
                                qk = [qo.key]
                                if lev == 1:
                                    Qt = AM[c].ap(0, 64)[:, 0:64]
                                    qk = [qo.key, AM[c].key]
                                bk, kk_ = pb()
                                mm(bk[0:64, 0:64], Qt, Q, True, True, qk, [kk_])
                                if lev < 5:
                                    mm(bk[0:64, 64:128], Q, Qt, True, True, qk, [kk_])
                                    op('act', 'activation', out=qn.ap(0, 64), in_=bk[0:64, 0:128], func=AF.Copy, r=[kk_], w=[qn.key])
                                else:
                                    op('act', 'activation', out=qn.ap(0, 64)[:, 0:64], in_=bk[0:64, 0:64], func=AF.Copy, r=[kk_], w=[qn.key])
                                bk2, kk2 = pb()
                                mm(bk2[0:64, 0:64], qn.ap(0, 64)[:, 0:64], to.ap(0, 64), True, True, [qn.key, to.key], [kk2])
                                op('dve', 'tensor_tensor', out=tn.ap(0, 64), in0=bk2[0:64, 0:64], in1=to.ap(0, 64), op=ALU.add,
                                   r=[kk2, to.key], w=[tn.key])
                        TF = [TT[c][5 % 2] for c in range(8)]
                        if stop == 'mixC':
                            return finish()

                        Sst = arena[0:64, RST.o + h_ * 64:RST.o + h_ * 64 + 64]
                        ybk, ykk = ybank()
                        for c in range(8):
                            cs = slice(c * 64, (c + 1) * 64)
                            am, tmv = AM[c].ap(0, 64), TM[c].ap(0, 64)
                            Vt, Bet, Ket = tmv[:, 0:64], tmv[:, 64:128], tmv[:, 128:192]
                            w0b, ub = W0B[c % 2], UB[c % 2]
                            bk, kk_ = pb()
                            mm(bk[0:64, 0:64], A_('at')[:, cs], Sst, True, False, PREP + [RST.key], [kk_])
                            mm(bk[0:64, 0:64], am[:, 64:128], Vt, False, True, [AM[c].key, TM[c].key], [kk_])
                            op('act', 'activation', out=w0b.ap(0, 64), in_=bk[0:64, 0:64], func=AF.Copy, r=[kk_], w=[w0b.key])
                            mm(bk[0:64, 64:128], TF[c].ap(0, 64), w0b.ap(0, 64), True, True, [TF[c].key, w0b.key], [kk_])
                            op('dve', 'tensor_copy', out=ub.ap(0, 64), in_=bk[0:64, 64:128], r=[kk_], w=[ub.key])
                            yo = ybk[0:64, cs]
                            mm(yo, A_('rt')[:, cs], Sst, True, False, PREP + [RST.key], [ykk])
                            mm(yo, am[:, 128:192], ub.ap(0, 64), False, False, [AM[c].key, ub.key], [ykk])
                            mm(yo, am[:, 192:256], Vt, False, True, [AM[c].key, TM[c].key], [ykk])
                            mm(bk[0:64, 128:192], Bet, ub.ap(0, 64), True, False, [TM[c].key, ub.key], [kk_])
                            mm(bk[0:64, 128:192], Ket, Vt, False, True, [TM[c].key], [kk_])
                            op('dve', 'scalar_tensor_tensor', out=Sst, in0=Sst, scalar=PCB.ap(0, 64)[:, c:c + 1],
                               in1=bk[0:64, 128:192], op0=ALU.mult, op1=ALU.add, r=[RST.key, PCB.key, kk_], w=[RST.key])
                        ys3 = A_('ys').rearrange("p (c v) -> p c v", v=64)
                        sq3 = A_('sq').rearrange("p (c v) -> p c v", v=64)
                        sta = STA.ap(0, 64)
                        op('act', 'activation', out=A_('ys'), in_=ybk[0:64, :], func=AF.Copy, r=[ykk], w=K_('ys'))
                        op('act', 'activation', out=A_('sq'), in_=ybk[0:64, :], func=AF.Square, r=[ykk], w=K_('sq'))
                        op('dve', 'tensor_reduce', out=sta[:, 0:8], in_=ys3, axis=AX.X, op=ALU.add, r=K_('ys'), w=[STA.key])
                        op('dve', 'tensor_reduce', out=sta[:, 8:16], in_=sq3, axis=AX.X, op=ALU.add, r=K_('sq'), w=[STA.key])
                        op('dve', 'tensor_scalar', out=sta[:, 0:16], in0=sta[:, 0:16], scalar1=1.0 / 64.0, scalar2=None, op0=ALU.mult,
                           r=[STA.key], w=[STA.key])
                        op('dve', 'tensor_tensor', out=sta[:, 16:24], in0=sta[:, 0:8], in1=sta[:, 0:8], op=ALU.mult, r=[STA.key], w=[STA.key])
                        op('dve', 'tensor_tensor', out=sta[:, 8:16], in0=sta[:, 8:16], in1=sta[:, 16:24], op=ALU.subtract, r=[STA.key], w=[STA.key])
                        op('act', 'activation', out=sta[:, 8:16], in_=sta[:, 8:16], func=AF.Sqrt, bias=pcol('c_gneps', 0, 0, 64), scale=1.0,
                           r=[STA.key] + CK, w=[STA.key])
                        op('dve', 'reciprocal', out=sta[:, 8:16], in_=sta[:, 8:16], r=[STA.key], w=[STA.key])
                        op('dve', 'tensor_tensor', out=ys3, in0=ys3, in1=sta[:, 0:8].unsqueeze(2).to_broadcast([64, 8, 64]), op=ALU.subtract,
                           r=K_('ys') + [STA.key], w=K_('ys'))
                        op('dve', 'tensor_tensor', out=ys3, in0=ys3, in1=sta[:, 8:16].unsqueeze(2).to_broadcast([64, 8, 64]), op=ALU.mult,
                           r=K_('ys') + [STA.key], w=K_('ys'))
                        bk, kk_ = pb()
                        for c in range(8):
                            tr(bk[0:64, c * 64:(c + 1) * 64], ys3[:, c, :], id64, K_('ys') + CK, [kk_])
                        if stop == 'mixD':
                            return finish()
                        o16 = OUT16[h_ % 2]
                        op('act', 'activation', out=A_('ynT'), in_=bk[0:64, :], func=AF.Identity, bias=pc('gn_b'), scale=pc('gn_w'),
                           r=[kk_] + CK, w=K_('ynT'))
                        op('dve', 'tensor_tensor', out=A_('ynT'), in0=A_('ynT'), in1=A_('bv'), op=ALU.add, r=K_('ynT', 'bv'), w=K_('ynT'))
                        op('dve', 'tensor_tensor', out=o16.ap(0, 64), in0=A_('ynT'), in1=A_('g'), op=ALU.mult, r=K_('ynT', 'g'), w=[o16.key])
                        dma('sp', ymix[h_ * 64:(h_ + 1) * 64, tok0:tok0 + 512], o16.ap(0, 64), r=[o16.key], w=[('ymix', h_, tok0)])
                    if stop == 'mixE':
                        return finish()

                    for gh in range(4):
                        proj(xb, 1824 + gh * 64, 64, A_('r'), W['r'].key)
                        proj(xb, 2080 + gh * 64, 64, A_('k'), W['k'].key)
                        proj(xb, 2336 + gh * 128, 128, W['a'].ap(), W['a'].key)
                        proj(xb, 2864 + gh * 128, 128, W['g'].ap(), W['g'].key)
                        op('act', 'activation', out=W['g'].ap(), in_=W['g'].ap(), func=AF.Silu, r=K_('g'), w=K_('g'))
                        bk, kk_ = pb()
                        mm(bk[0:64, :], GKU.ap(0, 16)[:, gh * 64:gh * 64 + 64], A_('gkd', 16), True, True, [GKU.key] + K_('gkd'), [kk_])
                        op('dve', 'tensor_scalar', out=A_('lw'), in0=bk[0:64, :], scalar1=pcol(f'ngk_b{l}', gh, 0, 64), scalar2=-1.0,
                           op0=ALU.add, op1=ALU.mult, r=[kk_] + CK, w=K_('lw'))
                        op('act', 'activation', out=A_('lw'), in_=A_('lw'), func=AF.Exp, r=K_('lw'), w=K_('lw'))
                        op('act', 'activation', out=A_('lw'), in_=A_('lw'), func=AF.Ln, bias=pcol('c_one', 0, 0, 64), scale=1.0,
                           r=K_('lw') + CK, w=K_('lw'))
                        op('dve', 'tensor_scalar', out=A_('lw'), in0=A_('lw'), scalar1=-1.0 / 16.0, scalar2=None, op0=ALU.mult, r=K_('lw'), w=K_('lw'))
                        op('dve', 'tensor_tensor_scan', out=A_('cum'), data0=cst('keep', 512, 0, 64), data1=A_('lw'),
                           initial=0.0, op0=ALU.mult, op1=ALU.add, r=K_('lw') + CK, w=K_('cum'))
                        cum3 = A_('cum').rearrange("p (c k) -> p c k", k=64)
                        op('act', 'activation', out=A_('e1'), in_=A_('cum'), func=AF.Exp, r=K_('cum'), w=K_('e1'))
                        op('dve', 'scalar_tensor_tensor', out=A_('rt'), in0=A_('r'), scalar=0.125, in1=A_('e1'), op0=ALU.mult, op1=ALU.mult,
                           r=K_('r', 'e1'), w=K_('rt'))
                        op('act', 'activation', out=PCB.ap(0, 64), in_=cum3[:, :, 63], func=AF.Exp, r=K_('cum'), w=[PCB.key])
                        op('act', 'activation', out=A_('e2'), in_=A_('cum'), func=AF.Exp, scale=-1.0, r=K_('cum'), w=K_('e2'))
                        op('dve', 'tensor_tensor', out=A_('kt'), in0=A_('k'), in1=A_('e2'), op=ALU.mult, r=K_('k', 'e2'), w=K_('kt'))
                        e23 = A_('e2').rearrange("p (c k) -> p c k", k=64)
                        op('dve', 'tensor_tensor', out=e23, in0=cum3[:, :, 63:64].to_broadcast([64, 8, 64]), in1=cum3, op=ALU.subtract,
                           r=K_('cum'), w=K_('e2'))
                        op('act', 'activation', out=A_('e2'), in_=A_('e2'), func=AF.Exp, r=K_('e2'), w=K_('e2'))
                        op('dve', 'tensor_tensor', out=A_('kend'), in0=A_('k'), in1=A_('e2'), op=ALU.mult, r=K_('k', 'e2'), w=K_('kend'))
                        GP = K_('rt', 'kt', 'kend', 'a')
                        for c in range(8):
                            cs = slice(c * 64, (c + 1) * 64)
                            bk, kk_ = pb()
                            mm(bk[0:64, 0:64], A_('kt')[:, cs], A_('rt')[:, cs], True, True, GP, [kk_])
                            tr(bk[0:64, 64:192], W['a'].ap()[:, cs], ident, K_('a') + CK, [kk_])
                            tr(bk[0:64, 192:256], A_('kend')[:, cs], id64, K_('kend') + CK, [kk_])
                            op('dve', 'tensor_copy', out=TM[c].ap(0, 64), in_=bk[0:64, 64:256], r=[kk_], w=[TM[c].key])
                            op('dve', 'tensor_tensor', out=AM[c].ap(0, 64)[:, 0:64], in0=bk[0:64, 0:64], in1=cst('incl', 64, 0, 64), op=ALU.mult,
                               r=[kk_] + CK, w=[AM[c].key])
                        Gst = arena[0:64, GST.o + gh * 128:GST.o + gh * 128 + 128]
                        for half in range(2):
                            ybk, ykk = ybank()
                            for c4 in range(4):
                                c = half * 4 + c4
                                cs = slice(c * 64, (c + 1) * 64)
                                Vt, Ket = TM[c].ap(0, 64)[:, 0:128], TM[c].ap(0, 64)[:, 128:192]
                                yo = ybk[0:64, c4 * 128:(c4 + 1) * 128]
                                mm(yo, A_('rt')[:, cs], Gst, True, False, GP + [GST.key], [ykk])
                                mm(yo, AM[c].ap(0, 64)[:, 0:64], Vt, False, True, [AM[c].key, TM[c].key], [ykk])
                                bk, kk_ = pb()
                                mm(bk[0:64, 0:128], Ket, Vt, True, True, [TM[c].key], [kk_])
                                op('dve', 'scalar_tensor_tensor', out=Gst, in0=Gst, scalar=PCB.ap(0, 64)[:, c:c + 1], in1=bk[0:64, 0:128],
                                   op0=ALU.mult, op1=ALU.add, r=[GST.key, PCB.key, kk_], w=[GST.key])
                            ys3 = A_('ys').rearrange("p (c v) -> p c v", v=128)
                            sq3 = A_('sq').rearrange("p (c v) -> p c v", v=128)
                            sta = STA.ap(0, 64)
                            op('act', 'activation', out=A_('ys'), in_=ybk[0:64, :], func=AF.Copy, r=[ykk], w=K_('ys'))
                            op('act', 'activation', out=A_('sq'), in_=ybk[0:64, :], func=AF.Square, r=[ykk], w=K_('sq'))
                            op('dve', 'tensor_reduce', out=sta[:, 0:4], in_=sq3, axis=AX.X, op=ALU.add, r=K_('sq'), w=[STA.key])
                            op('act', 'activation', out=sta[:, 0:4], in_=sta[:, 0:4], func=AF.Sqrt, bias=pcol('c_rmseps', 0, 0, 64),
                               scale=1.0 / 128.0, r=[STA.key] + CK, w=[STA.key])
                            op('dve', 'reciprocal', out=sta[:, 0:4], in_=sta[:, 0:4], r=[STA.key], w=[STA.key])
                            op('dve', 'tensor_tensor', out=ys3, in0=ys3, in1=sta[:, 0:4].unsqueeze(2).to_broadcast([64, 4, 128]), op=ALU.mult,
                               r=K_('ys') + [STA.key], w=K_('ys'))
                            bk, kk_ = pb()
                            for c4 in range(4):
                                tr(bk[:, c4 * 64:(c4 + 1) * 64], ys3[:, c4, :], id64, K_('ys') + CK, [kk_])
                            op('act', 'activation', out=W['ynT'].ap()[:, half * 256:(half + 1) * 256], in_=bk[:, 0:256], func=AF.Copy,
                               r=[kk_], w=K_('ynT'))
                        o16 = OUT16[gh % 2]
                        op('dve', 'scalar_tensor_tensor', out=o16.ap(), in0=W['ynT'].ap(), scalar=pcol(f'gnorm_w{l}'), in1=W['g'].ap(),
                           op0=ALU.mult, op1=ALU.mult, r=K_('ynT', 'g') + CK, w=[o16.key])
                        dma('sp', ymix[(4 + gh) * 128:(5 + gh) * 128, tok0:tok0 + 512], o16.ap(), r=[o16.key], w=[('ymix', 8 + gh, tok0)])
            if stop == f'mix{l}':
                return finish()

            phase_reset()
            WO = Buf(4096, 'wo', BF16)
            wo3 = WO.ap().rearrange("p (c f) -> p c f", c=8)
            dma('pool', wo3, w_out[l].rearrange("(c p) f -> p c f", p=128), w=[WO.key])
            YM = [Buf(2048, 'ym', BF16), Buf(2048, 'ym', BF16)]
            Zb = [Buf(4096, 'Z'), Buf(4096, 'Z')]
            SQb = [Buf(4096, 'SQ'), Buf(4096, 'SQ')]
            T1b = [Buf(512, 'T1'), Buf(512, 'T1')]
            T2b = [Buf(512, 'T2'), Buf(512, 'T2')]
            for tt in range(8):
                tok0 = tt * 512
                ym, Z = YM[tt % 2], Zb[tt % 2]
                ym3 = ym.ap().rearrange("p (c t) -> p c t", c=8)
                z3 = Z.ap().rearrange("p (c t) -> p c t", c=8)
                dma('sp', ym3, ymix.rearrange("(c p) t -> p c t", p=128)[:, :, tok0:tok0 + 512], w=[ym.key])
                dma('sp', z3, xres.rearrange("(c p) t -> p c t", p=128)[:, :, tok0:tok0 + 512], w=[Z.key])
                for dc in range(8):
                    bk, kk_ = pb()
                    for fc in range(8):
                        mm(bk[:], wo3[:, fc, dc * 128:(dc + 1) * 128], ym3[:, fc, :], fc == 0, fc == 7, [WO.key, ym.key], [kk_])
                    op('dve', 'scalar_tensor_tensor', out=z3[:, dc, :], in0=z3[:, dc, :], scalar=ALPHA, in1=bk[:], op0=ALU.mult,
                       op1=ALU.add, r=[Z.key, kk_], w=[Z.key])
                layer_norm_tile(z3, Z.ap(), Z.key, f'ln1_w{l}', f'ln1_b{l}', tok0, (SQb[tt % 2], T1b[tt % 2], T2b[tt % 2]))
            if stop == f'ln1_{l}':
                return finish()

            phase_reset()
            RW = Buf(256, 'rw')
            rw3 = RW.ap().rearrange("p (c e) -> p c e", c=8)
            dma('sp', rw3, router_w[l].rearrange("(c p) e -> p c e", p=128), w=[RW.key])
            RBB = Buf(32, 'rbb')
            dma('sp', RBB.ap(), router_bb[l], w=[RBB.key])
            XR = [Buf(4096, 'xr'), Buf(4096, 'xr')]
            RT_ = [Buf(48, 'rt_') for _ in range(2)]
            G2B = [Buf(40, 'g2') for _ in range(2)]
            GSTG = [Buf(128, 'gstg') for _ in range(2)]
            for tt in range(8):
                tok0 = tt * 512
                xr = XR[tt % 2]
                xr3 = xr.ap().rearrange("p (c t) -> p c t", c=8)
                dma('sp', xr3, xres.rearrange("(c p) t -> p c t", p=128)[:, :, tok0:tok0 + 512], r=[('xres', tok0)], w=[xr.key])
                for s4 in range(4):
                    i = tt * 4 + s4
                    rt, G2, gs = RT_[i % 2], G2B[i % 2], GSTG[i % 2]
                    lg, t8, nmx = rt.ap()[:, 0:32], rt.ap()[:, 32:40], rt.ap()[:, 40:41]
                    g2, gsum = G2.ap()[:, 0:32], G2.ap()[:, 32:33]
                    bk, kk_ = pb()
                    for c in range(8):
                        mm(bk[:, 0:32], xr3[:, c, s4 * 128:(s4 + 1) * 128], rw3[:, c, :], c == 0, c == 7, [xr.key, RW.key], [kk_])
                    op('dve', 'tensor_tensor', out=lg, in0=bk[:, 0:32], in1=RBB.ap(), op=ALU.add, r=[kk_, RBB.key], w=[rt.key])
                    if stop == 'routerA':
                        return finish()
                    op('dve', 'max', out=t8, in_=lg, r=[rt.key], w=[rt.key])
                    op('dve', 'tensor_scalar', out=nmx, in0=t8[:, 0:1], scalar1=-1.0, scalar2=None, op0=ALU.mult, r=[rt.key], w=[rt.key])
                    op('dve', 'tensor_scalar', out=g2, in0=lg, scalar1=t8[:, 3:4], scalar2=None, op0=ALU.is_ge, r=[rt.key], w=[G2.key])
                    op('act', 'activation', out=lg, in_=lg, func=AF.Exp, bias=nmx, scale=1.0, r=[rt.key], w=[rt.key])
                    op('dve', 'tensor_tensor', out=g2, in0=g2, in1=lg, op=ALU.mult, r=[rt.key, G2.key], w=[G2.key])
                    op('dve', 'tensor_reduce', out=gsum, in_=g2, axis=AX.X, op=ALU.add, r=[G2.key], w=[G2.key])
                    op('dve', 'reciprocal', out=gsum, in_=gsum, r=[G2.key], w=[G2.key])
                    op('dve', 'tensor_scalar', out=g2, in0=g2, scalar1=gsum, scalar2=None, op0=ALU.mult, r=[G2.key], w=[G2.key])
                    if stop == 'routerB':
                        return finish()
                    bk, kk_ = pb()
                    S.add('pe', (lambda o, s_: (lambda h: h.transpose(out=o, in_=s_, identity=ident)))(bk[0:32, 0:128], g2),
                          reads=[G2.key] + CK, writes=[kk_])
                    op('act', 'activation', out=gs.ap(0, 32), in_=bk[0:32, 0:128], func=AF.Copy, r=[kk_], w=[gs.key])
                    dma('sp', gTd[:, i * 128:(i + 1) * 128], gs.ap(0, 32), r=[gs.key], w=[('gT', i // 16)])
                    if stop == 'routerC':
                        return finish()
                    if stop is not None and stop.startswith('routerN') and i == int(stop[7:]):
                        return finish()
            if stop == f'router{l}':
                return finish()

            phase_reset()
            psi['n'] = 8
            X16 = Buf(8192, 'x16h', BF16)
            x16h = X16.ap().rearrange("p (c t) -> p c t", c=8)
            YACC = Buf(16384, 'yacc')
            yacc3 = YACC.ap().rearrange("p (c t) -> p c t", c=8)
            W1P = [Buf(2048, 'w1p', BF16), Buf(2048, 'w1p', BF16)]
            W2B = Buf(4096, 'w2b', BF16)
            w2v = W2B.ap().rearrange("p (c f) -> p c f", c=8)
            A16 = Buf(8192, 'a16', BF16)
            a16 = A16.ap().rearrange("p (c t) -> p c t", c=8)
            GT = Buf(2048, 'gT')
            B2 = Buf(1024, 'b2')
            dma('sp', B2.ap(0, 32), exp_b2[l], w=[B2.key])
            TMP = [[Buf(512, 'mt') for _ in range(2)] for _ in range(2)]
            UPA = [Buf(512, 'upa') for _ in range(3)]
            GB = Buf(1024, 'gb', BF16)
            XRl = Buf(0, 'alias'); XRl.o, XRl.key, XRl.words = A16.o, A16.key, 4096
            SQl = Buf(0, 'alias'); SQl.o, SQl.key, SQl.words = A16.o + 4096, A16.key, 4096
            for b in range(2):
                t0 = b * SEQ
                dma('sp', x16h, x16.rearrange("(c p) t -> p c t", p=128)[:, :, t0:t0 + SEQ],
                    r=[('x16', t0 + k * 512) for k in range(4)], w=[X16.key])
                dma('sp', GT.ap(0, 32), gTd[:, t0:t0 + SEQ], r=[('gT', b)], w=[GT.key])
                for tl in range(4):
                    ts = slice(tl * 512, (tl + 1) * 512)
                    for dc in range(8):
                        bk, kk_ = pb()
                        mm(bk[:], B2.ap(0, 32)[:, dc * 128:(dc + 1) * 128], GT.ap(0, 32)[:, ts], True, True, [B2.key, GT.key], [kk_])
                        op('act', 'activation', out=yacc3[:, dc, ts], in_=bk[:], func=AF.Copy, r=[kk_], w=[(YACC.key, tl, dc)])
                for e in range(NE):
                    if not (SKIPW and e > 0):
                        dma('pool', w2v, exp_w2[l, e].rearrange("(c p) f -> p c f", p=128), w=[W2B.key])
                    w1e = exp_w1[l, e].rearrange("(c p) f -> p c f", p=128)
                    for tl in range(4):
                        ts = slice(tl * 512, (tl + 1) * 512)
                        bgb, kgb = pb()
                        mm(bgb[:], ident[0:32, e:e + 1].to_broadcast([32, 128]), GT.ap(0, 32)[:, ts], True, True, [GT.key] + CK, [kgb])
                        op('act', 'activation', out=GB.ap()[:, ts], in_=bgb[:], func=AF.Copy, r=[kgb], w=[GB.key])
                    prev = None
                    prev2 = None
                    for it in range(34):
                        cur = None
                        if it < 32:
                            pc, sub, tl = it // 8, (it // 4) % 2, it % 4
                            fc = pc * 2 + sub
                            w1p = W1P[(e * 4 + pc) % 2]
                            w1v = w1p.ap().rearrange("p (c f) -> p c f", c=8)
                            if sub == 0 and tl == 0 and not (SKIPW and e > 0):
                                dma('pool', w1v[:, :, 0:256], w1e[:, :, pc * 256:(pc + 1) * 256], w=[w1p.key])
                                dma('pool', w1v[:, :, 256:512], w1e[:, :, 1024 + pc * 256:1024 + (pc + 1) * 256], w=[w1p.key])
                            ts = slice(tl * 512, (tl + 1) * 512)
                            gtb, sgb = TMP[it % 2]
                            upa = UPA[it % 3]
                            bg, kg = pb()
                            for dc in range(8):
                                mm(bg[:], w1v[:, dc, sub * 128:(sub + 1) * 128], x16h[:, dc, ts], dc == 0, dc == 7, [w1p.key, X16.key], [kg])
                            bu, ku = pb()
                            for dc in range(8):
                                mm(bu[:], w1v[:, dc, 256 + sub * 128:256 + (sub + 1) * 128], x16h[:, dc, ts], dc == 0, dc == 7,
                                   [w1p.key, X16.key], [ku])
                            op('act', 'activation', out=upa.ap(), in_=bu[:], func=AF.Identity, bias=pcol(f'b1{l}', e * 16 + 8 + fc), scale=1.0,
                               r=[ku] + CK, w=[upa.key])
                            cur = (gtb, sgb, upa, fc, ts, bg, kg)
                        if prev is not None:
                            pg, psg, pup, pfc, pts, _, _ = prev
                            op('dve', 'tensor_tensor', out=psg.ap(), in0=psg.ap(), in1=pg.ap(), op=ALU.mult, r=[psg.key, pg.key], w=[psg.key])
                            op('dve', 'tensor_scalar', out=pup.ap(), in0=pup.ap(), scalar1=7.0, scalar2=-7.0, op0=ALU.min, op1=ALU.max,
                               r=[pup.key], w=[pup.key])
                        if prev2 is not None:
                            _, _, qup, qfc, qts, _, _ = prev2
                            op('dve', 'tensor_tensor', out=a16[:, qfc, qts], in0=qup.ap(), in1=GB.ap()[:, qts], op=ALU.mult, r=[qup.key, GB.key], w=[A16.key])
                        if cur is not None:
                            gtb, sgb, upa, fc, ts, bg, kg = cur
                            op('dve', 'tensor_scalar', out=gtb.ap(), in0=bg[:], scalar1=pcol(f'b1{l}', e * 16 + fc), scalar2=7.0,
                               op0=ALU.add, op1=ALU.min, r=[kg] + CK, w=[gtb.key])
                            op('act', 'activation', out=sgb.ap(), in_=gtb.ap(), func=AF.Sigmoid, scale=1.702, r=[gtb.key], w=[sgb.key])
                        if prev is not None:
                            pg, psg, pup, pfc, pts, _, _ = prev
                            op('dve', 'scalar_tensor_tensor', out=pup.ap(), in0=pup.ap(), scalar=1.0, in1=psg.ap(), op0=ALU.add, op1=ALU.mult,
                               r=[pup.key, psg.key], w=[pup.key])
                        prev2 = prev
                        prev = cur
                    for tl in range(4):
                        ts = slice(tl * 512, (tl + 1) * 512)
                        for dc in range(8):
                            bk, kk_ = pb()
                            for fc in range(8):
                                mm(bk[:], w2v[:, fc, dc * 128:(dc + 1) * 128], a16[:, fc, ts], fc == 0, fc == 7, [W2B.key, A16.key], [kk_])
                            op('dve', 'tensor_tensor', out=yacc3[:, dc, ts], in0=yacc3[:, dc, ts], in1=bk[:], op=ALU.add, r=[(YACC.key, tl, dc), kk_], w=[(YACC.key, tl, dc)])
                for tl in range(4):
                    tok0 = t0 + tl * 512
                    ts = slice(tl * 512, (tl + 1) * 512)
                    xr3 = XRl.ap().rearrange("p (c t) -> p c t", c=8)
                    dma('sp', xr3, xres.rearrange("(c p) t -> p c t", p=128)[:, :, tok0:tok0 + 512], r=[('xres', tok0)], w=[A16.key])
                    op('dve', 'scalar_tensor_tensor', out=xr3, in0=xr3, scalar=ALPHA, in1=yacc3[:, :, ts], op0=ALU.mult, op1=ALU.add,
                       r=[A16.key] + [(YACC.key, tl, dc) for dc in range(8)], w=[A16.key])
                    layer_norm_tile(xr3, XRl.ap(), A16.key, f'ln2_w{l}', f'ln2_b{l}', tok0, (SQl, TMP[0][0], TMP[0][1]), to_out=(l == 1))
            if stop == f'moe{l}':
                return finish()
        return finish()


_CACHE = {}


def _prep_common(inp):
    L = make_layout()
    f32 = lambda a: np.ascontiguousarray(np.asarray(a, np.float32))
    com = {
        "params": pack_params(inp, L),
        "consts": make_consts(),
        "router_bb": np.ascontiguousarray(np.broadcast_to(np.asarray(inp['router_b'], np.float32)[:, None, :], (2, 128, NE))),
    }
    for k in ["w_in", "rwkv_w_up", "rwkv_a_up", "rwkv_g_up", "rwkv_v_down", "rwkv_v_up", "gla_gk_up", "w_out", "router_w",
              "exp_w1", "exp_w2", "exp_b2"]:
        com[k] = f32(inp[k])
    return com


def kernel(**inputs):
    if 'nc' not in _CACHE:
        _CACHE['nc'] = build()
    nc = _CACHE['nc']
    com = _prep_common(inputs)
    x = np.asarray(inputs['x'], np.float32)
    in_maps = []
    for c in range(NCORES):
        xc = x[2 * c:2 * c + 2].reshape(TOK, D)
        m = dict(com)
        m["xT"] = np.ascontiguousarray(xc.T)
        in_maps.append(m)
    res = run_bass_kernel_spmd(nc, in_maps, core_ids=list(range(NCORES)))
    out = np.empty((16, SEQ, D), np.float32)
    for c in range(NCORES):
        out[2 * c:2 * c + 2] = np.asarray(res.results[c]["outT"]).T.reshape(2, SEQ, D)
    return out
```
